# Optimizing a Trainium2 kernel written in Bass

```python
import math
import jax, jax.numpy as jnp
from jax import lax
import numpy as np

D_MODEL = 2048
BATCH = 1
SEQ = 16384
DEPTH = 2

Q_BLOCK = 128
A_HEADS = 8
A_QK_DIM = 64
A_V_DIM = 2 * A_QK_DIM
B_HEADS = 8
B_Q_LORA = 512
B_KV_LORA = 512
B_NOPE_DIM = 128
B_ROPE_DIM = 64
B_V_DIM = 128
ROPE_THETA = 10000.0
C_HEADS = 16
C_HEAD_DIM = D_MODEL // C_HEADS
FFN_DIM = 5632
N_EXPERTS = 8
TOP_K = 2
EXPERT_DIM = 5632
EVEN_IN_DIM = 2 * A_HEADS * 2 * A_QK_DIM + A_HEADS * A_V_DIM + B_Q_LORA + B_KV_LORA + B_ROPE_DIM
EVEN_MIX_DIM = A_HEADS * A_V_DIM + B_HEADS * B_V_DIM
ODD_IN_DIM = 3 * C_HEADS * C_HEAD_DIM + C_HEADS
N_EVEN = (DEPTH + 1) // 2
N_ODD = DEPTH // 2

kernel_name = "hybrid_diffattn_mla_fox_moe"


def rms_norm(x, g, eps=1e-6):
    xf = x.astype(jnp.float32)
    y = xf * lax.rsqrt(jnp.mean(xf * xf, axis=-1, keepdims=True) + eps)
    return (y * g.astype(jnp.float32)).astype(x.dtype)


def split_sizes(x, sizes):
    idx, acc = [], 0
    for s in sizes[:-1]:
        acc += s
        idx.append(acc)
    return jnp.split(x, idx, axis=-1)


def rope_tables(seq, dim):
    inv = ROPE_THETA ** (-jnp.arange(0, dim, 2, dtype=jnp.float32) / dim)
    ang = jnp.arange(seq, dtype=jnp.float32)[:, None] * inv[None, :]
    return jnp.cos(ang), jnp.sin(ang)


def apply_rope(x, cos, sin):
    xf = x.astype(jnp.float32)
    x1, x2 = jnp.split(xf, 2, axis=-1)
    return jnp.concatenate([x1 * cos - x2 * sin, x2 * cos + x1 * sin], axis=-1).astype(x.dtype)


def block_offsets(i, seq):
    qpos = i * Q_BLOCK + jnp.arange(Q_BLOCK)
    return qpos[:, None] - jnp.arange(seq)[None, :]


def sweep_query_blocks(block_fn, seq):
    out = lax.map(block_fn, jnp.arange(seq // Q_BLOCK))
    nb, b, qb, h, d = out.shape
    return jnp.moveaxis(out, 0, 1).reshape(b, nb * qb, h, d)


def diff_attention(q, k, v, lam, slopes):
    seq = q.shape[1]
    scale = A_QK_DIM ** -0.5

    def block(i):
        qb = lax.dynamic_slice_in_dim(q, i * Q_BLOCK, Q_BLOCK, axis=1)
        dist = block_offsets(i, seq)
        logits = jnp.einsum('bqhmd,bkhmd->bhmqk', qb, k).astype(jnp.float32) * scale
        logits = logits - slopes[:, None, None, None] * dist.astype(jnp.float32)
        logits = jnp.where(dist >= 0, logits, -jnp.inf)
        p = jax.nn.softmax(logits, axis=-1)
        w = (p[:, :, 0] - lam * p[:, :, 1]).astype(v.dtype)
        return jnp.einsum('bhqk,bkhd->bqhd', w, v)

    return sweep_query_blocks(block, seq)


def mla_attention(q_nope, q_pe, k_nope, k_pe, v):
    seq = q_nope.shape[1]
    scale = (B_NOPE_DIM + B_ROPE_DIM) ** -0.5

    def block(i):
        qn = lax.dynamic_slice_in_dim(q_nope, i * Q_BLOCK, Q_BLOCK, axis=1)
        qp = lax.dynamic_slice_in_dim(q_pe, i * Q_BLOCK, Q_BLOCK, axis=1)
        dist = block_offsets(i, seq)
        logits = (jnp.einsum('bqhd,bkhd->bhqk', qn, k_nope)
                  + jnp.einsum('bqhr,bkr->bhqk', qp, k_pe)).astype(jnp.float32) * scale
        logits = jnp.where(dist >= 0, logits, -jnp.inf)
        p = jax.nn.softmax(logits, axis=-1).astype(v.dtype)
        return jnp.einsum('bhqk,bkhd->bqhd', p, v)

    return sweep_query_blocks(block, seq)


def forgetting_attention(q, k, v, log_f):
    seq = q.shape[1]
    scale = C_HEAD_DIM ** -0.5
    cum = jnp.cumsum(log_f, axis=1).transpose(0, 2, 1)

    def block(i):
        qb = lax.dynamic_slice_in_dim(q, i * Q_BLOCK, Q_BLOCK, axis=1)
        cq = lax.dynamic_slice_in_dim(cum, i * Q_BLOCK, Q_BLOCK, axis=2)
        dist = block_offsets(i, seq)
        logits = jnp.einsum('bqhd,bkhd->bhqk', qb, k).astype(jnp.float32) * scale
        logits = logits + (cq[..., :, None] - cum[..., None, :])
        logits = jnp.where(dist >= 0, logits, -jnp.inf)
        p = jax.nn.softmax(logits, axis=-1).astype(v.dtype)
        return jnp.einsum('bhqk,bkhd->bqhd', p, v)

    return sweep_query_blocks(block, seq)


def even_mixer(h, w_in, q_norm, w_uq, kv_norm, w_ukv, lq1, lk1, lq2, lk2, subln, w_out, lambda_init):
    b, s, _ = h.shape
    proj = h @ w_in
    aq, ak, av, cq, ckv, kpe = split_sizes(
        proj, [A_HEADS * 2 * A_QK_DIM, A_HEADS * 2 * A_QK_DIM, A_HEADS * A_V_DIM,
               B_Q_LORA, B_KV_LORA, B_ROPE_DIM])
    aq = aq.reshape(b, s, A_HEADS, 2, A_QK_DIM)
    ak = ak.reshape(b, s, A_HEADS, 2, A_QK_DIM)
    av = av.reshape(b, s, A_HEADS, A_V_DIM)
    f32 = jnp.float32
    lam = (jnp.exp(jnp.sum(lq1.astype(f32) * lk1.astype(f32)))
           - jnp.exp(jnp.sum(lq2.astype(f32) * lk2.astype(f32))) + lambda_init)
    slopes = jnp.exp2(-8.0 * jnp.arange(1, A_HEADS + 1, dtype=f32) / A_HEADS)
    oa = diff_attention(aq, ak, av, lam, slopes)
    oa = (rms_norm(oa, subln, eps=1e-5) * (1.0 - lambda_init)).reshape(b, s, A_HEADS * A_V_DIM)
    cos, sin = rope_tables(s, B_ROPE_DIM)
    qb = (rms_norm(cq, q_norm) @ w_uq).reshape(b, s, B_HEADS, B_NOPE_DIM + B_ROPE_DIM)
    q_nope, q_pe = jnp.split(qb, [B_NOPE_DIM], axis=-1)
    q_pe = apply_rope(q_pe, cos[None, :, None, :], sin[None, :, None, :])
    kv = (rms_norm(ckv, kv_norm) @ w_ukv).reshape(b, s, B_HEADS, B_NOPE_DIM + B_V_DIM)
    k_nope, vb = jnp.split(kv, [B_NOPE_DIM], axis=-1)
    k_pe = apply_rope(kpe, cos[None], sin[None])
    ob = mla_attention(q_nope, q_pe, k_nope, k_pe, vb).reshape(b, s, B_HEADS * B_V_DIM)
    return jnp.concatenate([oa, ob], axis=-1) @ w_out


def odd_mixer(h, w_in, forget_bias, w_out):
    b, s, _ = h.shape
    proj = h @ w_in
    width = C_HEADS * C_HEAD_DIM
    q, k, v, f = split_sizes(proj, [width, width, width, C_HEADS])
    log_f = jax.nn.log_sigmoid((f + forget_bias).astype(jnp.float32))
    shp = (b, s, C_HEADS, C_HEAD_DIM)
    o = forgetting_attention(q.reshape(shp), k.reshape(shp), v.reshape(shp), log_f)
    return o.reshape(b, s, width) @ w_out


def swiglu(h, w_gate, w_up, w_down):
    return (jax.nn.silu(h @ w_gate) * (h @ w_up)) @ w_down


def moe_swiglu(h, router, w_gate, w_up, w_down):
    logits = (h @ router).astype(jnp.float32)
    probs = jax.nn.softmax(logits, axis=-1)
    top_p, top_i = lax.top_k(probs, TOP_K)
    top_p = top_p / jnp.sum(top_p, axis=-1, keepdims=True)
    gates = jnp.sum(jax.nn.one_hot(top_i, N_EXPERTS, dtype=jnp.float32) * top_p[..., None], axis=-2)
    out = jnp.zeros_like(h)
    for e in range(N_EXPERTS):
        out = out + gates[..., e:e + 1].astype(h.dtype) * swiglu(h, w_gate[e], w_up[e], w_down[e])
    return out


def setup_inputs(seed: int = 0) -> dict:
    key = jax.random.key(seed)
    ks = iter(jax.random.split(key, 32))
    f32 = jnp.float32

    def w(shape, fan_in):
        return jax.random.normal(next(ks), shape, f32) * fan_in ** -0.5

    def gain(shape):
        return 1.0 + 0.02 * jax.random.normal(next(ks), shape, f32)

    E, O, D = N_EVEN, N_ODD, D_MODEL
    return {
        "x": jax.random.normal(next(ks), (BATCH, SEQ, D), f32),
        "ev_attn_norm": gain((E, D)),
        "ev_w_in": w((E, D, EVEN_IN_DIM), D),
        "ev_q_norm": gain((E, B_Q_LORA)),
        "ev_w_uq": w((E, B_Q_LORA, B_HEADS * (B_NOPE_DIM + B_ROPE_DIM)), B_Q_LORA),
        "ev_kv_norm": gain((E, B_KV_LORA)),
        "ev_w_ukv": w((E, B_KV_LORA, B_HEADS * (B_NOPE_DIM + B_V_DIM)), B_KV_LORA),
        "ev_lambda_q1": 0.1 * jax.random.normal(next(ks), (E, A_QK_DIM), f32),
        "ev_lambda_k1": 0.1 * jax.random.normal(next(ks), (E, A_QK_DIM), f32),
        "ev_lambda_q2": 0.1 * jax.random.normal(next(ks), (E, A_QK_DIM), f32),
        "ev_lambda_k2": 0.1 * jax.random.normal(next(ks), (E, A_QK_DIM), f32),
        "ev_subln": gain((E, A_V_DIM)),
        "ev_w_out": w((E, EVEN_MIX_DIM, D), EVEN_MIX_DIM),
        "ev_ffn_norm": gain((E, D)),
        "ev_ffn_w_gate": w((E, D, FFN_DIM), D),
        "ev_ffn_w_up": w((E, D, FFN_DIM), D),
        "ev_ffn_w_down": w((E, FFN_DIM, D), FFN_DIM),
        "od_attn_norm": gain((O, D)),
        "od_w_in": w((O, D, ODD_IN_DIM), D),
        "od_forget_bias": jax.random.uniform(next(ks), (O, C_HEADS), f32, 1.0, 4.0),
        "od_w_out": w((O, C_HEADS * C_HEAD_DIM, D), C_HEADS * C_HEAD_DIM),
        "od_ffn_norm": gain((O, D)),
        "od_router": w((O, D, N_EXPERTS), D),
        "od_moe_w_gate": w((O, N_EXPERTS, D, EXPERT_DIM), D),
        "od_moe_w_up": w((O, N_EXPERTS, D, EXPERT_DIM), D),
        "od_moe_w_down": w((O, N_EXPERTS, EXPERT_DIM, D), EXPERT_DIM),
        "final_norm": gain((D,)),
    }


def reference(x, ev_attn_norm, ev_w_in, ev_q_norm, ev_w_uq, ev_kv_norm, ev_w_ukv,
              ev_lambda_q1, ev_lambda_k1, ev_lambda_q2, ev_lambda_k2, ev_subln, ev_w_out,
              ev_ffn_norm, ev_ffn_w_gate, ev_ffn_w_up, ev_ffn_w_down,
              od_attn_norm, od_w_in, od_forget_bias, od_w_out,
              od_ffn_norm, od_router, od_moe_w_gate, od_moe_w_up, od_moe_w_down,
              final_norm):
    h = x
    for layer in range(DEPTH):
        j = layer // 2
        if layer % 2 == 0:
            lambda_init = 0.8 - 0.6 * math.exp(-0.3 * layer)
            h = h + even_mixer(rms_norm(h, ev_attn_norm[j]), ev_w_in[j], ev_q_norm[j], ev_w_uq[j],
                               ev_kv_norm[j], ev_w_ukv[j], ev_lambda_q1[j], ev_lambda_k1[j],
                               ev_lambda_q2[j], ev_lambda_k2[j], ev_subln[j], ev_w_out[j], lambda_init)
            h = h + swiglu(rms_norm(h, ev_ffn_norm[j]), ev_ffn_w_gate[j], ev_ffn_w_up[j], ev_ffn_w_down[j])
        else:
            h = h + odd_mixer(rms_norm(h, od_attn_norm[j]), od_w_in[j], od_forget_bias[j], od_w_out[j])
            h = h + moe_swiglu(rms_norm(h, od_ffn_norm[j]), od_router[j], od_moe_w_gate[j],
                               od_moe_w_up[j], od_moe_w_down[j])
    return rms_norm(h, final_norm)
```

```python
import numpy as np
import ml_dtypes
from contextlib import ExitStack
import concourse.bass as bass
import concourse.mybir as mybir
from concourse.bass_utils import run_bass_kernel_spmd

F32 = mybir.dt.float32
BF16 = mybir.dt.bfloat16
U8 = mybir.dt.uint8
AF = mybir.ActivationFunctionType
ALU = mybir.AluOpType
AX = mybir.AxisListType
NCORE = 8
D = 2048
KC = D // 128
CH = 16
BIGM = 3.0e38


class Buf:
    __slots__ = ("ws", "r", "const", "pre", "excl")
    reg = []

    def __init__(self, const=False, excl=False):
        self.excl = excl
        self.ws = []
        self.r = []
        self.pre = []
        self.const = const
        Buf.reg.append(self)


class Ins:
    __slots__ = ("eng", "fn", "deps", "inc", "cnt", "dma", "k", "cc")


ENGS = ["pe", "act", "dve", "pool", "sp"]
NDS = 8


class Sched:
    def __init__(self):
        self.q = {e: [] for e in ENGS}
        self.nd = {e: 0 for e in ENGS}
        self.ncc = 0

    def op(self, eng, fn, r=(), w=(), pw=(), dma=False, cc=False):
        ins = Ins()
        ins.eng = eng
        ins.fn = fn
        ins.dma = dma
        ins.cc = -1
        ins.inc = False
        ins.cnt = 0
        ins.k = 0
        deps = {}
        if any(b.excl for b in r):
            w = tuple(w) + tuple(b for b in r if b.excl)
            r = tuple(b for b in r if not b.excl)
        for b in r:
            for x in b.ws:
                deps[id(x)] = x
        for b in w:
            for x in b.ws:
                deps[id(x)] = x
            for x in b.r:
                deps[id(x)] = x
        for b in pw:
            for x in b.r:
                deps[id(x)] = x
            for x in b.pre:
                deps[id(x)] = x
        ins.deps = [d for d in deps.values() if (d.dma or d.cc >= 0 or d.eng != eng or eng != "pe") and d is not ins]
        for d in ins.deps:
            if not d.dma and d.cc < 0:
                d.inc = True
        for b in r:
            if not b.const:
                b.r.append(ins)
        for b in w:
            b.pre = [x for x in (b.ws + b.r) if x is not ins]
            b.ws = [ins]
            b.r = []
        for b in pw:
            b.ws.append(ins)
        if dma:
            ins.k = self.nd[eng]
            self.nd[eng] += 1
        if cc:
            ins.cc = self.ncc
            self.ncc += 1
        self.q[eng].append(ins)
        return ins

    def barrier(self):
        lasts = []
        for e in ENGS:
            n = 0
            got_c = False
            for x in reversed(self.q[e]):
                if x.fn is None:
                    continue
                if x.dma:
                    if n < NDS:
                        lasts.append(x)
                        n += 1
                elif x.cc >= 0:
                    lasts.append(x)
                elif not got_c:
                    lasts.append(x)
                    got_c = True
                if n >= NDS and got_c:
                    break
        for e in ENGS:
            ins = self.op(e, None)
            ins.deps = [d for d in lasts if (d.dma or d.cc >= 0 or d.eng != e or e != "pe")]
            for d in ins.deps:
                if not d.dma and d.cc < 0:
                    d.inc = True
        for b in Buf.reg:
            b.ws = []
            b.r = []
            b.pre = []

    def emit(self, block, esem, dsem, ccsems):
        for e in ENGS:
            c = 0
            for ins in self.q[e]:
                if ins.inc:
                    c += 1
                ins.cnt = c
        sched = self

        def run(e, eh):
            waited = {}

            def wait(key, sem, val):
                if waited.get(key, 0) >= val:
                    return
                eh.wait_ge(sem, val)
                waited[key] = val

            for ins in sched.q[e]:
                for d in ins.deps:
                    if d.dma:
                        wait((d.eng, d.k % NDS), dsem[d.eng][d.k % NDS], 16 * (d.k // NDS + 1))
                    elif d.cc >= 0:
                        wait(("cc", d.cc), ccsems[d.cc], 1)
                    else:
                        wait(d.eng, esem[d.eng], d.cnt)
                if ins.dma and ins.k >= NDS:
                    wait((e, ins.k % NDS), dsem[e][ins.k % NDS], 16 * (ins.k // NDS))
                if ins.fn is None:
                    continue
                bi = ins.fn(eh)
                if ins.dma:
                    bi.then_inc(dsem[e][ins.k % NDS], 16)
                elif ins.cc >= 0:
                    bi.then_inc(ccsems[ins.cc])
                elif ins.inc:
                    bi.then_inc(esem[e], 1)

        block.tensor(lambda eh: run("pe", eh))
        block.scalar(lambda eh: run("act", eh))
        block.vector(lambda eh: run("dve", eh))
        block.gpsimd(lambda eh: run("pool", eh))
        block.sync(lambda eh: run("sp", eh))


def build(S, FF, FE, NE, stop_after=None, taps=()):
    Buf.reg = []
    TL = S // NCORE
    NB = TL // 128
    NT = TL // 512
    NKB = NCORE * NB
    GF = 256
    nc = bass.Bass("TRN2", target_bir_lowering=False)
    sc = Sched()

    def din(name, shape, dt=F32):
        return nc.dram_tensor(name, list(shape), dt, kind="ExternalInput")

    def dscr(name, shape, dt):
        return nc.dram_tensor(name, list(shape), dt)

    x_d = din("x", [TL, D])
    gains_d = din("gains", [5, D])
    w0_d = din("w0", [D, 4224])
    qn_d = din("qn", [128, 4])
    kvn_d = din("kvn", [128, 4])
    wuq_d = din("wuq", [512, 2048])
    wukv_d = din("wukv", [512, 2048])
    lam_d = din("lam", [1, 256])
    subln_d = din("subln", [1, 128])
    wo0_d = din("wo0", [D, D])
    wg0_d = din("wg0", [1, D, FF])
    wu0_d = din("wu0", [1, D, FF])
    wd0_d = din("wd0", [1, FF, D])
    w1_d = din("w1", [D, 6160])
    fb_d = din("fb", [1, 16])
    wo1_d = din("wo1", [D, D])
    rt_d = din("rt", [NE, D])
    wg1_d = din("wg1", [NE, D, FE])
    wu1_d = din("wu1", [NE, D, FE])
    wd1_d = din("wd1", [NE, FE, D])
    identb_d = din("identb", [128, 128], BF16)
    identf_d = din("identf", [128, 128])
    dmaskb_d = din("dmaskb", [128, 8 * 128], BF16)
    dmaskf_d = din("dmaskf", [128, 8 * 128])
    tri3_d = din("tri3", [128, 3 * 128])
    dmaskn_d = din("dmaskn", [128, 8 * 128])
    cosT_d = din("cosT", [64, TL])
    sinT_d = din("sinT", [64, TL])
    bqd_d = din("bqd", [8, TL])
    bkd_d = din("bkd", [128, 8 * NKB])
    y_d = nc.dram_tensor("y", [TL, D], F32, kind="ExternalOutput")
    tap_d = {}
    for nm, shp, dt in taps:
        tap_d[nm] = nc.dram_tensor("tap_" + nm, list(shp), dt, kind="ExternalOutput")
    hA_d = dscr("hA", [TL, D], F32)
    hB_d = dscr("hB", [TL, D], F32)
    hC_d = dscr("hC", [TL, D], F32)
    kt0_d = dscr("kt0", [2112, TL], BF16)
    kt0g_d = dscr("kt0g", [NCORE * 2112, TL], BF16)
    v0_d = dscr("v0", [16 * TL, 128], BF16)
    v0g_d = dscr("v0g", [NCORE * 16 * TL, 128], BF16)
    qt0_d = dscr("qt0", [2560, TL], BF16)
    ot_d = dscr("ot", [2048, TL], BF16)
    kt1_d = dscr("kt1", [2048, TL], BF16)
    kt1g_d = dscr("kt1g", [NCORE * 2048, TL], BF16)
    v1_d = dscr("v1", [16 * TL, 128], BF16)
    v1g_d = dscr("v1g", [NCORE * 16 * TL, 128], BF16)
    qt1_d = dscr("qt1", [2048, TL], BF16)
    lf_d = dscr("lf", [TL, 16], F32)
    lfg_d = dscr("lfg", [NCORE * TL, 16], F32)
    cumq_d = dscr("cumq", [16, TL], F32)

    B_hA, B_hB, B_hC = Buf(), Buf(), Buf()
    B_kt0, B_kt0g, B_v0, B_v0g, B_qt0, B_ot = Buf(), Buf(), Buf(), Buf(), Buf(), Buf()
    B_kt1, B_kt1g, B_v1, B_v1g, B_qt1, B_lf, B_lfg, B_cumq = Buf(), Buf(), Buf(), Buf(), Buf(), Buf(), Buf(), Buf()
    B_y = Buf()
    B_tap = Buf()

    ARENA = 204 * 1024
    es = ExitStack()
    arena = es.enter_context(nc.sbuf_tensor("arena", [128, ARENA], U8))[:, :]
    pss = [es.enter_context(nc.psum_tensor("ps%d" % i, [128, 512], F32))[:, :] for i in range(8)]
    esem = {e: es.enter_context(nc.semaphore("es_" + e)) for e in ENGS}
    dsem = {e: [es.enter_context(nc.semaphore("ds_%s%d" % (e, j))) for j in range(NDS)] for e in ENGS}
    ccsems = [es.enter_context(nc.semaphore("cc%d" % j)) for j in range(6)]
    B_ps = [Buf(excl=True) for _ in range(8)]

    class Ar:
        def __init__(self, base=0):
            self.off = base

        def take(self, nbytes, dt):
            nbytes = (nbytes + 63) // 64 * 64
            v = arena[:, self.off:self.off + nbytes].bitcast(dt)
            self.off += nbytes
            assert self.off <= ARENA, (self.off, ARENA)
            return v

    def dma(eng, out, in_, r=(), w=(), pw=()):
        def f(e):
            return e.dma_start(out=out, in_=in_)
        return sc.op(eng, f, r, w, pw, dma=True)

    def mm(out, lhsT, rhs, st, sp, r=(), w=(), pw=(), skip=False):
        def f(e):
            if skip:
                return e.matmul(out, lhsT, rhs, start=st, stop=sp, skip_group_check=True)
            return e.matmul(out, lhsT, rhs, start=st, stop=sp)
        return sc.op("pe", f, r, w, pw)

    def tr(out, in_, ident, r=(), w=(), pw=()):
        def f(e):
            return e.transpose(out, in_, ident)
        return sc.op("pe", f, r, w, pw)

    def act(out, in_, func, r=(), w=(), pw=(), bias=None, scale=1.0):
        def f(e):
            if bias is None:
                return e.activation(out=out, in_=in_, func=func, scale=scale)
            return e.activation(out=out, in_=in_, func=func, bias=bias, scale=scale)
        return sc.op("act", f, r, w, pw)

    def tt(eng, out, a, b, op, r=(), w=(), pw=()):
        def f(e):
            return e.tensor_tensor(out=out, in0=a, in1=b, op=op)
        return sc.op(eng, f, r, w, pw)

    def ts(eng, out, a, s1, s2, op0, op1, r=(), w=(), pw=()):
        def f(e):
            if s2 is None:
                return e.tensor_scalar(out=out, in0=a, scalar1=s1, scalar2=None, op0=op0)
            return e.tensor_scalar(out=out, in0=a, scalar1=s1, scalar2=s2, op0=op0, op1=op1)
        return sc.op(eng, f, r, w, pw)

    def stt(out, a, s, b, op0, op1, r=(), w=(), pw=()):
        def f(e):
            return e.scalar_tensor_tensor(out=out, in0=a, scalar=s, in1=b, op0=op0, op1=op1)
        return sc.op("dve", f, r, w, pw)

    def cp(eng, out, in_, r=(), w=(), pw=()):
        if eng == "act":
            def f(e):
                return e.copy(out=out, in_=in_)
        else:
            def f(e):
                return e.tensor_copy(out=out, in_=in_)
        return sc.op(eng, f, r, w, pw)

    def red(out, in_, op, r=(), w=(), pw=()):
        def f(e):
            return e.tensor_reduce(out=out, in_=in_, axis=AX.X, op=op)
        return sc.op("dve", f, r, w, pw)

    def recip(out, in_, r=(), w=(), pw=()):
        def f(e):
            return e.reciprocal(out=out, in_=in_)
        return sc.op("dve", f, r, w, pw)

    def memset(eng, ap, val, w=(), pw=()):
        def f(e):
            return e.memset(ap, val)
        return sc.op(eng, f, (), w, pw)

    def allgather(src, dst, r, w):
        def f(e):
            return e.collective_compute("AllGather", ALU.bypass, replica_groups=[list(range(NCORE))],
                                        ins=[src.ap().opt()], outs=[dst.ap().opt()])
        return sc.op("pool", f, r, w, cc=True)

    def tapout(nm, src_ap, bsrc, dst_ap=None):
        if nm in tap_d:
            dma("sp", tap_d[nm].ap() if dst_ap is None else dst_ap, src_ap, (bsrc,), (), (B_tap,))

    pa = Ar(0)
    identb = pa.take(256, BF16)
    identf = pa.take(512, F32)
    onesf = pa.take(512, F32)
    onesb = pa.take(256, BF16)
    dmaskb = pa.take(2048, BF16)
    neghalf = pa.take(2048, F32)
    gain_bc = pa.take(8192, F32)
    gates = pa.take(NB * 8 * 4, F32).rearrange("p (b e) -> p b e", e=8)
    B_const = Buf(const=True)
    B_gain = Buf()
    B_gates = Buf()
    dma("sp", identb, identb_d.ap(), (), (B_const,))
    dma("sp", identf, identf_d.ap(), (), (), (B_const,))
    dma("sp", dmaskb, dmaskb_d.ap(), (), (), (B_const,))
    memset("dve", onesf, 1.0, (), (B_const,))
    memset("dve", onesb, 1.0, (), (B_const,))
    memset("dve", neghalf, -0.5, (), (B_const,))
    PBASE = pa.off

    def load_gain(i):
        dma("sp", gain_bc, gains_d[i:i + 1, :].partition_broadcast(128)[:, 0, :], (), (B_gain,))

    def rsqrt(out, in_, mul, eps, n, r, w):
        ts("dve", out, in_, mul, eps, ALU.mult, ALU.add, r, w)
        tt("pool", out, out, neghalf[:, 0:n], ALU.pow, list(w) + [B_const], w)

    pscnt = [0]

    def nextps():
        i = pscnt[0] % 4
        pscnt[0] += 1
        return i

    evc = [0]

    def evac_copy(out, pi, M, w=(), pw=()):
        e = "act" if evc[0] % 2 == 0 else "dve"
        evc[0] += 1
        cp(e, out, pss[pi][0:M, :], (B_ps[pi],), w, pw)

    class NormBufs:
        def __init__(self, a):
            self.sq = a.take(8192, F32)
            self.B_sq = Buf()
            self.hn = [a.take(4096, BF16) for _ in range(2)]
            self.B_hn = [Buf(), Buf()]
            self.st = [a.take(64, F32) for _ in range(2)]
            self.B_st = [Buf(), Buf()]

    def norm_from(nb, hb, bh, hnT, bhnT, b, k, fp32_out=None):
        j = k % 2
        tt("dve", nb.sq, hb, hb, ALU.mult, (bh,), (nb.B_sq,))
        red(nb.st[j][:, 0:1], nb.sq, ALU.add, (nb.B_sq,), (nb.B_st[j],))
        rsqrt(nb.st[j][:, 1:2], nb.st[j][:, 0:1], 1.0 / D, 1e-6, 1, (nb.B_st[j],), (nb.B_st[j],))
        if fp32_out is not None:
            o32, b32 = fp32_out
            stt(o32, hb, nb.st[j][:, 1:2], gain_bc, ALU.mult, ALU.mult, (bh, nb.B_st[j], B_gain), (b32,))
            if hnT is None:
                return
            cp("act", nb.hn[j], o32, (b32,), (nb.B_hn[j],))
        else:
            stt(nb.hn[j], hb, nb.st[j][:, 1:2], gain_bc, ALU.mult, ALU.mult, (bh, nb.B_st[j], B_gain), (nb.B_hn[j],))
        for half in range(2):
            pb = 6 + half
            pv = pss[pb].bitcast(BF16)
            for c in range(8):
                cc = half * 8 + c
                tr(pv[:, c * 128:(c + 1) * 128], nb.hn[j][:, cc * 128:(cc + 1) * 128], identb,
                   (nb.B_hn[j], B_const), (B_ps[pb],))
            cp("act" if half == 0 else "dve", hnT[:, half * 8:(half + 1) * 8, b * 128:(b + 1) * 128],
               pv[:, 0:1024].rearrange("p (c n) -> p c n", c=8), (B_ps[pb],), (), (bhnT,))

    class WRing:
        def __init__(self, a, nk, ncols, nslots=2):
            self.bufs = [a.take(nk * ncols * 2, BF16).rearrange("p (c n) -> p c n", c=nk) for _ in range(nslots)]
            self.B = [Buf() for _ in range(nslots)]
            self.n = 0
            self.nk = nk

        def load(self, src_fn, ncols, nk=None):
            nk = self.nk if nk is None else nk
            j = self.n % len(self.bufs)
            self.n += 1
            half = max(1, nk // 2)
            first = True
            for c0 in range(0, nk, half):
                if first:
                    dma("pool", self.bufs[j][:, c0:c0 + half, 0:ncols], src_fn(c0, c0 + half), (), (self.B[j],))
                    first = False
                else:
                    dma("pool", self.bufs[j][:, c0:c0 + half, 0:ncols], src_fn(c0, c0 + half), (), (), (self.B[j],))
            return self.bufs[j], self.B[j]

    a = Ar(PBASE)
    hnT = a.take(KC * TL * 2, BF16).rearrange("p (c n) -> p c n", c=KC)
    B_hnT = Buf()
    A_MARK = a.off
    xt = [a.take(8192, F32) for _ in range(2)]
    B_xt = [Buf(), Buf()]
    nbA = NormBufs(a)
    load_gain(0)
    for b in range(NB):
        j = b % 2
        dma("sp", xt[j], x_d[b * 128:(b + 1) * 128, :], (), (B_xt[j],))
        norm_from(nbA, xt[j], B_xt[j], hnT, B_hnT, b, b)
    if "hnT0" in tap_d:
        tapout("hnT0", hnT, B_hnT, tap_d["hnT0"].ap().rearrange("p (c n) -> p c n", c=KC))

    sc.barrier()
    if stop_after == "A":
        return nc, sc, es, esem, dsem, ccsems

    def proj_fm(wt, bw, col, M, nk, rhs_fn, brhs, tti):
        pi = nextps()
        for kc in range(nk):
            mm(pss[pi][0:M, :], wt[:, kc, col:col + M], rhs_fn(kc, tti), kc == 0, kc == nk - 1, (bw, brhs), (B_ps[pi],))
        return pi

    def hn_rhs(kc, tti):
        return hnT[:, kc, tti * 512:(tti + 1) * 512]

    a = Ar(A_MARK)
    wr = WRing(a, KC, 512)
    stg = [a.take(TL * 2, BF16) for _ in range(2)]
    B_stg = [Buf(), Buf()]
    vstg = [a.take(1024 * 2, BF16) for _ in range(2)]
    B_vstg = [Buf(), Buf()]
    cgT = [a.take(4 * TL * 2, BF16).rearrange("p (c n) -> p c n", c=4) for _ in range(2)]
    B_cgT = [Buf(), Buf()]
    rbc = [a.take(TL * 4, F32) for _ in range(2)]
    B_rbc = [Buf(), Buf()]
    rcol = a.take(max(NB, 16) * 4, F32)
    B_rcol = Buf()
    sqt = [a.take(2048, F32) for _ in range(2)]
    B_sqt = [Buf(), Buf()]
    nrm = a.take(64, F32)
    B_nrm = Buf()
    ropeT = a.take(2 * TL * 4, F32).rearrange("p (c n) -> p c n", c=2)
    B_rope = Buf(const=True)
    tmp1 = [a.take(2048, F32) for _ in range(2)]
    B_tmp1 = [Buf(), Buf()]
    dma("sp", nrm[:, 0:4], qn_d.ap(), (), (B_nrm,))
    dma("sp", nrm[:, 4:8], kvn_d.ap(), (), (), (B_nrm,))
    dma("sp", ropeT[0:64, 0, :], cosT_d.ap(), (), (B_rope,))
    dma("sp", ropeT[0:64, 1, :], sinT_d.ap(), (), (), (B_rope,))
    w0v = w0_d.ap().rearrange("(c p) n -> p c n", p=128)
    scnt = [0]
    vcnt = [0]

    def qk_groups(wv, col0, ngrp, dst_of_head):
        for grp in range(ngrp):
            wt, bw = wr.load(lambda c0, c1, g=grp: wv[:, c0:c1, col0 + g * 512:col0 + (g + 1) * 512], 512)
            for hh in range(4):
                head = grp * 4 + hh
                j = scnt[0] % 2
                scnt[0] += 1
                for tti in range(NT):
                    pi = proj_fm(wt, bw, hh * 128, 128, KC, hn_rhs, B_hnT, tti)
                    evac_copy(stg[j][:, tti * 512:(tti + 1) * 512], pi, 128, (B_stg[j],) if tti == 0 else (),
                              () if tti == 0 else (B_stg[j],))
                dst, dbuf = dst_of_head(head)
                dma("sp", dst, stg[j], (B_stg[j],), (), (dbuf,))

    def v_groups(wv, col0, vdst_view, h0, bdst, lhs_fn, blhs, nk, scale_col=None):
        wts = []
        for grp in range(2):
            wts.append(wr.load(lambda c0, c1, g=grp: wv[:, c0:c1, col0 + g * 512:col0 + (g + 1) * 512], 512, nk=nk))
        for b in range(NB):
            j = vcnt[0] % 2
            vcnt[0] += 1
            for grp in range(2):
                wt, bw = wts[grp]
                pi = nextps()
                for kc in range(nk):
                    mm(pss[pi][:, :], lhs_fn(kc, b), wt[:, kc, 0:512], kc == 0, kc == nk - 1, (bw, blhs), (B_ps[pi],))
                wl = (B_vstg[j],) if grp == 0 else ()
                pl = () if grp == 0 else (B_vstg[j],)
                if scale_col is None:
                    evac_copy(vstg[j][:, grp * 512:(grp + 1) * 512], pi, 128, wl, pl)
                else:
                    ts("dve", vstg[j][:, grp * 512:(grp + 1) * 512], pss[pi], scale_col(b), None, ALU.mult, None,
                       (B_ps[pi], B_rcol), wl, pl)
            dma("sp", vdst_view[b * 128:(b + 1) * 128, h0:h0 + 8, :], vstg[j].rearrange("p (h d) -> p h d", h=8),
                (B_vstg[j],), (), (bdst,))

    v0v = v0_d.ap().rearrange("(h t) d -> t h d", h=16)
    qk_groups(w0v, 0, 2, lambda head: (qt0_d[head * 128:(head + 1) * 128, :], B_qt0))
    qk_groups(w0v, 1024, 2, lambda head: (kt0_d[head * 128:(head + 1) * 128, :], B_kt0))
    v_groups(w0v, 2048, v0v, 0, B_v0, lambda kc, b: hnT[:, kc, b * 128:(b + 1) * 128], B_hnT, KC)
    memset("dve", pss[5], 0.0, (B_ps[5],))
    for which in range(2):
        wt, bw = wr.load(lambda c0, c1, g=which: w0v[:, c0:c1, 3072 + g * 512:3072 + (g + 1) * 512], 512)
        for tti in range(NT):
            pis = 4
            for fc in range(4):
                pi = proj_fm(wt, bw, fc * 128, 128, KC, hn_rhs, B_hnT, tti)
                j = (tti * 4 + fc) % 2
                act(sqt[j], pss[pi], AF.Square, (B_ps[pi],), (B_sqt[j],))
                ts("dve", cgT[which][:, fc, tti * 512:(tti + 1) * 512], pss[pi], nrm[:, which * 4 + fc:which * 4 + fc + 1], None,
                   ALU.mult, None, (B_ps[pi], B_nrm), (), (B_cgT[which],))
                mm(pss[pis], onesf, sqt[j], fc == 0, fc == 3, (B_const, B_sqt[j]), (B_ps[pis],))
                if which == 1:
                    for bb in range(4):
                        col = tti * 4 + bb
                        mm(pss[5][:, col:col + 1], sqt[j][:, bb * 128:(bb + 1) * 128], onesf[:, 0:1], False, False,
                           (B_sqt[j], B_const), (B_ps[5],), skip=True)
            rsqrt(rbc[which][:, tti * 512:(tti + 1) * 512], pss[pis], 1.0 / 512, 1e-6, 512, (B_ps[pis],), (B_rbc[which],)
                  if tti == 0 else (B_rbc[which],))
    rsqrt(rcol[:, 0:NB], pss[5][:, 0:NB], 1.0 / 512, 1e-6, NB, (B_ps[5],), (B_rcol,))
    wt, bw = wr.load(lambda c0, c1: w0v[:, c0:c1, 4096:4224], 128)
    j = scnt[0] % 2
    scnt[0] += 1

    def rope_evac(p1, p2, sl, out_ap, bout, first, rb=None):
        tt("dve", tmp1[0][0:64, :], pss[p1][0:64, :], ropeT[0:64, 0, sl], ALU.mult, (B_ps[p1], B_rope), (B_tmp1[0],))
        tt("dve", tmp1[1][0:64, :], pss[p2][0:64, :], ropeT[0:64, 1, sl], ALU.mult, (B_ps[p2], B_rope), (B_tmp1[1],))
        wl = (bout,) if first else ()
        pl = () if first else (bout,)
        if rb is None:
            tt("dve", out_ap, tmp1[0][0:64, :], tmp1[1][0:64, :], ALU.add, (B_tmp1[0], B_tmp1[1]), wl, pl)
        else:
            tt("dve", tmp1[0][0:64, :], tmp1[0][0:64, :], tmp1[1][0:64, :], ALU.add, (B_tmp1[0], B_tmp1[1]), (B_tmp1[0],))
            tt("dve", out_ap, tmp1[0][0:64, :], rb, ALU.mult, (B_tmp1[0], B_rbc[0]), wl, pl)

    for tti in range(NT):
        p1 = proj_fm(wt, bw, 0, 64, KC, hn_rhs, B_hnT, tti)
        p2 = proj_fm(wt, bw, 64, 64, KC, hn_rhs, B_hnT, tti)
        sl = slice(tti * 512, (tti + 1) * 512)
        rope_evac(p1, p2, sl, stg[j][0:64, sl], B_stg[j], tti == 0)
    dma("sp", kt0_d[2048:2112, :], stg[j][0:64, :], (B_stg[j],), (), (B_kt0,))
    wuqv = wuq_d.ap().rearrange("(c p) n -> p c n", p=128)
    wukvv = wukv_d.ap().rearrange("(c p) n -> p c n", p=128)

    def cq_rhs(kc, tti):
        return cgT[0][:, kc, tti * 512:(tti + 1) * 512]

    def ckv_rhs(kc, tti):
        return cgT[1][:, kc, tti * 512:(tti + 1) * 512]

    for which, wv, rhsf, dst, dbuf in ((0, wuqv, cq_rhs, qt0_d, B_qt0), (1, wukvv, ckv_rhs, kt0_d, B_kt0)):
        for grp in range(2):
            wt, bw = wr.load(lambda c0, c1, g=grp, wv=wv: wv[:, c0:c1, g * 512:(g + 1) * 512], 512, nk=4)
            for hh in range(4):
                head = grp * 4 + hh
                j = scnt[0] % 2
                scnt[0] += 1
                for tti in range(NT):
                    pi = proj_fm(wt, bw, hh * 128, 128, 4, rhsf, B_cgT[which], tti)
                    sl = slice(tti * 512, (tti + 1) * 512)
                    tt("dve", stg[j][:, sl], pss[pi], rbc[which][:, sl], ALU.mult, (B_ps[pi], B_rbc[which]),
                       (B_stg[j],) if tti == 0 else (), () if tti == 0 else (B_stg[j],))
                dma("sp", dst[1024 + head * 128:1024 + (head + 1) * 128, :], stg[j], (B_stg[j],), (), (dbuf,))
    wtp, bwp = wr.load(lambda c0, c1: wuqv[:, c0:c1, 1024:1536], 512, nk=4)
    wtq, bwq = wr.load(lambda c0, c1: wuqv[:, c0:c1, 1536:2048], 512, nk=4)
    for head in range(8):
        j = scnt[0] % 2
        scnt[0] += 1
        for tti in range(NT):
            p1 = proj_fm(wtp, bwp, head * 64, 64, 4, cq_rhs, B_cgT[0], tti)
            p2 = proj_fm(wtq, bwq, head * 64, 64, 4, cq_rhs, B_cgT[0], tti)
            sl = slice(tti * 512, (tti + 1) * 512)
            rope_evac(p1, p2, sl, stg[j][0:64, sl], B_stg[j], tti == 0, rb=rbc[0][0:64, sl])
        dma("sp", qt0_d[2048 + head * 64:2048 + (head + 1) * 64, :], stg[j][0:64, :], (B_stg[j],), (), (B_qt0,))
    v_groups(wukvv, 1024, v0v, 8, B_v0, lambda kc, b: cgT[1][:, kc, b * 128:(b + 1) * 128], B_cgT[1], 4,
             scale_col=lambda b: rcol[:, b:b + 1])
    tapout("qt0", qt0_d.ap(), B_qt0)
    tapout("kt0", kt0_d.ap(), B_kt0)
    tapout("v0", v0_d.ap(), B_v0)
    sc.barrier()
    if stop_after == "B":
        return nc, sc, es, esem, dsem, ccsems

    allgather(kt0_d, kt0g_d, (B_kt0,), (B_kt0g,))
    allgather(v0_d, v0g_d, (B_v0,), (B_v0g,))

    def attention(layer):
        a = Ar(PBASE)
        RING = 3
        ktr = [a.take(8 * 128 * 2, BF16).rearrange("p (r n) -> p r n", r=8) for _ in range(RING)]
        kt2r = [a.take(8 * 128 * 2, BF16).rearrange("p (r n) -> p r n", r=8) for _ in range(RING)]
        vr = [a.take(8 * 128 * 2, BF16).rearrange("p (r n) -> p r n", r=8) for _ in range(RING)]
        B_kv = [Buf() for _ in range(RING)]
        qT = [a.take(TL * 2, BF16) for _ in range(2)]
        q2T = [a.take(TL * 2, BF16) for _ in range(2)]
        bq = [a.take(TL * 4, F32) for _ in range(2)]
        B_q = [Buf(), Buf()]
        sbuf_ = [a.take(2048, F32) for _ in range(2)]
        B_sb = [Buf(), Buf()]
        pT = [a.take(1024, BF16) for _ in range(3)]
        B_pT = [Buf() for _ in range(3)]
        of0 = a.take(NT * 512 * 4, F32).rearrange("p (j q d) -> p j q d", j=NT, q=4)
        B_of0 = [Buf() for _ in range(NT)]
        of1 = [a.take(2048, F32).rearrange("p (q d) -> p q d", q=4) for _ in range(2)]
        B_of1 = [Buf(), Buf()]
        onb = [a.take(1024, BF16).rearrange("p (q d) -> p q d", q=4) for _ in range(2)]
        B_onb = [Buf(), Buf()]
        sqe = a.take(2048, F32).rearrange("p (q d) -> p q d", q=4)
        B_sqe = Buf()
        rc = [a.take(64, F32) for _ in range(2)]
        B_rc = [Buf(), Buf()]
        oTs = [a.take(TL * 2, BF16) for _ in range(2)]
        B_oTs = [Buf(), Buf()]
        small = a.take(1024, F32)
        B_small = Buf()
        sublnsc = a.take(512, F32)
        B_subln = Buf()
        if layer == 0:
            bk = a.take(8 * NKB * 4, F32).rearrange("p (h k) -> p h k", h=8)
        else:
            bk = a.take(NKB * 16 * 4, F32).rearrange("p (m r h) -> p m r h", r=8, h=16)
        B_bk = Buf()
        B_ocnt = [0]
        dmaskn = a.take(4096, F32)
        B_dmn = Buf(const=True)
        dma("sp", dmaskn, dmaskn_d.ap(), (), (B_dmn,))

        if layer == 0:
            dma("sp", bk, bkd_d.ap().rearrange("p (h k) -> p h k", h=8), (), (B_bk,))
            lam_init = 0.2
            dma("sp", small[:, 0:256], lam_d[0:1, :].partition_broadcast(128)[:, 0, :], (), (B_small,))
            dma("sp", sublnsc, subln_d[0:1, :].partition_broadcast(128)[:, 0, :], (), (B_subln,))
            lamb = a.take(64, F32)
            B_lam = Buf()
            tt("dve", small[:, 0:64], small[:, 0:64], small[:, 64:128], ALU.mult, (B_small,), (B_small,))
            tt("dve", small[:, 128:192], small[:, 128:192], small[:, 192:256], ALU.mult, (B_small,), (B_small,))
            red(lamb[:, 0:1], small[:, 0:64], ALU.add, (B_small,), (B_lam,))
            red(lamb[:, 1:2], small[:, 128:192], ALU.add, (B_small,), (B_lam,))
            act(lamb[:, 2:4], lamb[:, 0:2], AF.Exp, (B_lam,), (B_lam,))
            tt("dve", lamb[:, 4:5], lamb[:, 3:4], lamb[:, 2:3], ALU.subtract, (B_lam,), (B_lam,))
            ts("dve", lamb[:, 5:6], lamb[:, 4:5], -lam_init, None, ALU.add, None, (B_lam,), (B_lam,))
            ts("dve", sublnsc, sublnsc, 1.0 - lam_init, None, ALU.mult, None, (B_subln,), (B_subln,))
        else:
            pass

        hcount = [0]
        kvcount = [0]

        def run_head(spec):
            hj = hcount[0] % 2
            hcount[0] += 1
            ktg, B_ktg, vg, B_vg, KR = spec["kv"]
            dma("sp", qT[hj], spec["q"], (spec["bq_"],), (B_q[hj],))
            if spec["q2"] is not None:
                dma("sp", q2T[hj][0:64, :], spec["q2"], (spec["bq_"],), (), (B_q[hj],))
            if spec["bias"] is not None:
                dma("sp", bq[hj], spec["bias"][0], (spec["bias"][2],), (), (B_q[hj],))
            ktv = ktg.ap().rearrange("(r q) t -> q r t", r=NCORE)
            vv = vg.ap().rearrange("(r h t) d -> t r h d", r=NCORE, h=16)
            nmaps = len(spec["maps"])
            oj = B_ocnt[0] % 2
            B_ocnt[0] += 1
            for mi, (prow, dk) in enumerate(spec["maps"]):
                pend = []
                stepn = [0]
                for J_ in range(NT):
                    memset("dve", pss[2 + J_], 0.0, (B_ps[2 + J_],))
                memset("dve", pss[6][:, 0:16], 0.0, (B_ps[6],))

                def flush(pend):
                    for fn in pend:
                        fn()
                    del pend[:]

                for m in range(NB):
                    slot = kvcount[0] % RING
                    kvcount[0] += 1
                    r0 = spec["krow0"]
                    dma("sp", ktr[slot], ktv[r0:r0 + 128, :, m * 128:(m + 1) * 128], (B_ktg,), (B_kv[slot],))
                    if spec["k2row0"] is not None:
                        r2 = spec["k2row0"]
                        dma("sp", kt2r[slot][0:64], ktv[r2:r2 + 64, :, m * 128:(m + 1) * 128], (B_ktg,), (), (B_kv[slot],))
                    dma("sp", vr[slot], vv[m * 128:(m + 1) * 128, :, spec["vh"], :], (B_vg,), (), (B_kv[slot],))
                    J0 = m // 4
                    for r in range(NCORE):
                        for J in range(J0, NT):
                            i = m - 4 * J
                            qoff = i * 128 if i > 0 else 0
                            diag = 0 <= i <= 3
                            n = stepn[0]
                            stepn[0] += 1
                            sb = n % 2
                            pb = n % 3
                            qs = slice(J * 512 + qoff, (J + 1) * 512)
                            mm(pss[sb][:, qoff:512], ktr[slot][prow:prow + dk, r, :], qT[hj][prow:prow + dk, qs], True,
                               spec["k2row0"] is None, (B_kv[slot], B_q[hj]), (B_ps[sb],))
                            if spec["k2row0"] is not None:
                                mm(pss[sb][:, qoff:512], kt2r[slot][0:64, r, :], q2T[hj][0:64, qs], False, True,
                                   (B_kv[slot], B_q[hj]), (B_ps[sb],))
                            flush(pend)
                            if spec["bias"] is not None:
                                stt(sbuf_[sb][:, qoff:512], pss[sb][:, qoff:512], spec["scale"], bq[hj][:, qs], ALU.mult, ALU.add,
                                    (B_ps[sb], B_q[hj]), (B_sb[sb],))
                                if diag:
                                    tt("dve", sbuf_[sb][:, qoff:qoff + 128], sbuf_[sb][:, qoff:qoff + 128], dmaskn[:, r * 128:(r + 1) * 128],
                                       ALU.add, (B_sb[sb], B_dmn), (B_sb[sb],))
                                act(pT[pb][:, qoff:512], sbuf_[sb][:, qoff:512], AF.Exp, (B_sb[sb], B_bk), (B_pT[pb],),
                                    bias=spec["bias"][1](m, r))
                            else:
                                act(pT[pb][:, qoff:512], pss[sb][:, qoff:512], AF.Exp, (B_ps[sb],), (B_pT[pb],), scale=spec["scale"])
                                if diag:
                                    tt("pool", pT[pb][:, qoff:qoff + 128], pT[pb][:, qoff:qoff + 128], dmaskb[:, r * 128:(r + 1) * 128],
                                       ALU.mult, (B_pT[pb], B_const), (B_pT[pb],))

                            def pv(J=J, i=i, qoff=qoff, pb=pb, slot=slot, m=m, r=r):
                                for qb in range(qoff // 128, 4):
                                    first = (m == 0 and r == 0)
                                    last = (m == 4 * J + qb and r == NCORE - 1)
                                    mm(pss[2 + J][:, qb * 128:(qb + 1) * 128], pT[pb][:, qb * 128:(qb + 1) * 128], vr[slot][:, r, :],
                                       False, False, (B_pT[pb], B_kv[slot]), (B_ps[2 + J],), skip=True)
                                    mm(pss[6][:, J * 4 + qb:J * 4 + qb + 1], pT[pb][:, qb * 128:(qb + 1) * 128], onesb[:, 0:1],
                                       False, False, (B_pT[pb], B_const), (B_ps[6],), skip=True)
                            pend.append(pv)
                    if m % 4 == 3:
                        flush(pend)
                        Jd = m // 4
                        epilogue(spec, mi, nmaps, Jd, oj)
            dma("sp", ot_d[spec["orow"]:spec["orow"] + 128, :], oTs[oj], (B_oTs[oj],), (), (B_ot,))

        ecount = [0]

        def epilogue(spec, mi, nmaps, J, oj):
            k = ecount[0] % 2
            ecount[0] += 1
            recip(rc[k][:, 0:4], pss[6][:, J * 4:J * 4 + 4], (B_ps[6],), (B_rc[k],))
            if nmaps == 2 and mi == 0:
                for qb in range(4):
                    ts("dve", of0[:, J, qb, :], pss[2 + J][:, qb * 128:(qb + 1) * 128], rc[k][:, qb:qb + 1], None, ALU.mult, None,
                       (B_ps[2 + J], B_rc[k]), (B_of0[J],) if qb == 0 else (), () if qb == 0 else (B_of0[J],))
                return
            if nmaps == 2:
                for qb in range(4):
                    ts("dve", of1[k][:, qb, :], pss[2 + J][:, qb * 128:(qb + 1) * 128], rc[k][:, qb:qb + 1], None, ALU.mult, None,
                       (B_ps[2 + J], B_rc[k]), (B_of1[k],) if qb == 0 else (), () if qb == 0 else (B_of1[k],))
                stt(of1[k], of1[k], lamb[:, 5:6], of0[:, J], ALU.mult, ALU.add, (B_of1[k], B_of0[J], B_lam), (B_of1[k],))
                tt("dve", sqe, of1[k], of1[k], ALU.mult, (B_of1[k],), (B_sqe,))
                red(rc[k][:, 4:8], sqe, ALU.add, (B_sqe,), (B_rc[k],))
                rsqrt(rc[k][:, 8:12], rc[k][:, 4:8], 1.0 / 128, 1e-5, 4, (B_rc[k],), (B_rc[k],))
                for qb in range(4):
                    stt(onb[k][:, qb, :], of1[k][:, qb, :], rc[k][:, 8 + qb:9 + qb], sublnsc[:, 0:128], ALU.mult, ALU.mult,
                        (B_of1[k], B_rc[k], B_subln), (B_onb[k],) if qb == 0 else (), () if qb == 0 else (B_onb[k],))
            else:
                for qb in range(4):
                    ts("dve", onb[k][:, qb, :], pss[2 + J][:, qb * 128:(qb + 1) * 128], rc[k][:, qb:qb + 1], None, ALU.mult, None,
                       (B_ps[2 + J], B_rc[k]), (B_onb[k],) if qb == 0 else (), () if qb == 0 else (B_onb[k],))
            pv7 = pss[7].bitcast(BF16)
            for qb in range(4):
                tr(pv7[:, qb * 128:(qb + 1) * 128], onb[k][:, qb, :], identb, (B_onb[k], B_const), (B_ps[7],))
            cp("act", oTs[oj][:, J * 512:(J + 1) * 512], pv7[:, 0:512], (B_ps[7],), (B_oTs[oj],) if J == 0 else (),
               () if J == 0 else (B_oTs[oj],))

        if layer == 0:
            bqv = bqd_d.ap()
            for hd in range(8):
                run_head(dict(q=qt0_d[hd * 128:(hd + 1) * 128, :], q2=None, bq_=B_qt0, kv=(kt0g_d, B_kt0g, v0g_d, B_v0g, 2112),
                              krow0=hd * 128, k2row0=None, vh=hd, maps=[(0, 64), (64, 64)], scale=64 ** -0.5,
                              bias=(bqv[hd:hd + 1, :].partition_broadcast(128)[:, 0, :], (lambda m, r, hd=hd: bk[:, hd, m * 8 + r:m * 8 + r + 1]),
                                    B_const), orow=hd * 128))
            for hd in range(8):
                run_head(dict(q=qt0_d[1024 + hd * 128:1024 + (hd + 1) * 128, :], q2=qt0_d[2048 + hd * 64:2048 + (hd + 1) * 64, :], bq_=B_qt0,
                              kv=(kt0g_d, B_kt0g, v0g_d, B_v0g, 2112), krow0=1024 + hd * 128, k2row0=2048, vh=8 + hd,
                              maps=[(0, 128)], scale=192 ** -0.5, bias=None, orow=1024 + hd * 128))
        else:
            LF = a.take(NKB * 16 * 4, F32).rearrange("p (r m h) -> p r m h", r=8, h=16)
            B_LF = Buf()
            tri3 = a.take(384 * 4, F32)
            dmf = a.take(1024 * 4, F32)
            B_tri = Buf(const=True)
            totS = a.take(NB * 16 * 4, F32).rearrange("p (m h) -> p m h", h=16)
            pre = a.take(NB * 16 * 4, F32).rearrange("p (m h) -> p m h", h=16)
            B_pre = Buf()
            cumQ = a.take(NB * 16 * 4, F32).rearrange("p (m h) -> p m h", h=16)
            B_cumQ = Buf()
            cqT = a.take(TL * 4, F32)
            B_cqT = Buf()
            dma("sp", LF, lfg_d.ap().rearrange("(r m p) h -> p r m h", r=8, p=128), (B_lfg,), (B_LF,))
            dma("sp", tri3, tri3_d.ap(), (), (B_tri,))
            dma("sp", dmf, dmaskf_d.ap(), (), (), (B_tri,))
            for m in range(NB):
                for r2 in range(8):
                    mm(pss[0][:, m * 16:(m + 1) * 16], onesf, LF[:, r2, m, :], r2 == 0, r2 == 7, (B_const, B_LF), (B_ps[0],))
            cp("dve", totS, pss[0][:, 0:NB * 16].rearrange("p (m h) -> p m h", h=16), (B_ps[0],), (B_pre,))
            memset("dve", pre[:, 0, :], 0.0, (), (B_pre,))
            for m in range(1, NB):
                tt("dve", pre[:, m, :], pre[:, m - 1, :], totS[:, m - 1, :], ALU.add, (B_pre,), (B_pre,))
            for m in range(NB):
                pb_ = 1 + (m % 2)
                for r in range(8):
                    for r2 in range(8):
                        sel = 0 if r2 < r else (1 if r2 == r else 2)
                        mm(pss[pb_][:, r * 16:(r + 1) * 16], tri3[:, sel * 128:(sel + 1) * 128], LF[:, r2, m, :], r2 == 0, r2 == 7,
                           (B_tri, B_LF), (B_ps[pb_],))
                for r in range(8):
                    stt(bk[:, m, r, :], pss[pb_][:, r * 16:(r + 1) * 16], -1.0, pre[:, m, :], ALU.mult, ALU.subtract,
                        (B_ps[pb_], B_pre), (), (B_bk,))
            for m in range(NB):
                for r2 in range(8):
                    mm(pss[3][:, m * 16:(m + 1) * 16], dmf[:, r2 * 128:(r2 + 1) * 128], LF[:, r2, m, :], r2 == 0, r2 == 7,
                       (B_tri, B_LF), (B_ps[3],))
            tt("dve", cumQ, pss[3][:, 0:NB * 16].rearrange("p (m h) -> p m h", h=16), pre, ALU.add, (B_ps[3], B_pre), (B_cumQ,))
            for m0 in range(0, NB, 4):
                for mi_ in range(4):
                    m = m0 + mi_
                    tr(pss[4][0:16, mi_ * 128:(mi_ + 1) * 128], cumQ[:, m, :], identf, (B_cumQ, B_const), (B_ps[4],))
                cp("act", cqT[0:16, m0 * 128:(m0 + 4) * 128], pss[4][0:16, :], (B_ps[4],), (), (B_cqT,))
            dma("sp", cumq_d.ap(), cqT[0:16, :], (B_cqT,), (B_cumq,))
            tapout("cumq", cumq_d.ap(), B_cumq)
            if "negcumk" in tap_d:
                tapout("negcumk", bk.rearrange("p m r h -> p (m r h)"), B_bk)
            cqv = cumq_d.ap()
            for hd in range(16):
                run_head(dict(q=qt1_d[hd * 128:(hd + 1) * 128, :], q2=None, bq_=B_qt1, kv=(kt1g_d, B_kt1g, v1g_d, B_v1g, 2048),
                              krow0=hd * 128, k2row0=None, vh=hd, maps=[(0, 128)], scale=128 ** -0.5,
                              bias=(cqv[hd:hd + 1, :].partition_broadcast(128)[:, 0, :], (lambda m, r, hd=hd: bk[:, m, r, hd:hd + 1]),
                                    B_cumq), orow=hd * 128))
        return None

    attention(0)
    tapout("ot0", ot_d.ap(), B_ot)
    sc.barrier()
    if stop_after == "D":
        return nc, sc, es, esem, dsem, ccsems

    def outproj(wo_d, hsrc_d, bsrc, hdst_d, bdst):
        a = Ar(PBASE)
        wo = a.take(KC * 2048 * 2, BF16).rearrange("p (c n) -> p c n", c=KC)
        B_wo = Buf()
        ott = [a.take(KC * 128 * 2, BF16).rearrange("p (c n) -> p c n", c=KC) for _ in range(2)]
        B_ott = [Buf(), Buf()]
        hres = [a.take(8192, F32) for _ in range(2)]
        B_hres = [Buf(), Buf()]
        wov = wo_d.ap().rearrange("(c p) n -> p c n", p=128)
        for c0 in range(0, KC, 4):
            dma("pool", wo[:, c0:c0 + 4, :], wov[:, c0:c0 + 4, :], (), (B_wo,) if c0 == 0 else (), () if c0 == 0 else (B_wo,))
        otv = ot_d.ap().rearrange("(c p) t -> p c t", p=128)
        for b in range(NB):
            j = b % 2
            dma("sp", ott[j], otv[:, :, b * 128:(b + 1) * 128], (B_ot,), (B_ott[j],))
            dma("sp", hres[j], hsrc_d[b * 128:(b + 1) * 128, :], (bsrc,) if bsrc is not None else (), (B_hres[j],))
            for ct in range(4):
                pi = nextps()
                for c in range(KC):
                    mm(pss[pi], ott[j][:, c, :], wo[:, c, ct * 512:(ct + 1) * 512], c == 0, c == KC - 1, (B_ott[j], B_wo), (B_ps[pi],))
                tt("dve", hres[j][:, ct * 512:(ct + 1) * 512], hres[j][:, ct * 512:(ct + 1) * 512], pss[pi], ALU.add,
                   (B_ps[pi], B_hres[j]), (B_hres[j],))
            dma("sp", hdst_d[b * 128:(b + 1) * 128, :], hres[j], (B_hres[j],), (), (bdst,))

    outproj(wo0_d, x_d, None, hA_d, B_hA)
    tapout("hA", hA_d.ap(), B_hA)
    sc.barrier()
    if stop_after == "E":
        return nc, sc, es, esem, dsem, ccsems

    def ffn(hsrc_d, bsrc, hdst_d, bdst, wg_d, wu_d, wd_d, NEx, Fdim, gain_idx, moe, final):
        T = min(1024, TL)
        NTT = TL // T
        TB = T // 128
        NH = T // 512
        NG = Fdim // GF
        FCN = GF // 128
        a = Ar(PBASE)
        hnTt = a.take(KC * T * 2, BF16).rearrange("p (c n) -> p c n", c=KC)
        B_hnTt = Buf()
        acc = a.take(TB * 2048 * 4, F32).rearrange("p (b n) -> p b n", b=TB)
        B_acc = [Buf() for _ in range(TB)]
        wgr = [a.take(KC * GF * 2, BF16).rearrange("p (c n) -> p c n", c=KC) for _ in range(2)]
        wur = [a.take(KC * GF * 2, BF16).rearrange("p (c n) -> p c n", c=KC) for _ in range(2)]
        B_wgu = [Buf(), Buf()]
        wdr = a.take(FCN * 2048 * 2, BF16).rearrange("p (c n) -> p c n", c=FCN)
        B_wd = Buf()
        GT = [a.take(FCN * T * 2, BF16).rearrange("p (c n) -> p c n", c=FCN) for _ in range(2)]
        B_GT = [Buf(), Buf()]
        sg = [a.take(2048, F32) for _ in range(2)]
        B_sg = [Buf(), Buf()]
        xt_ = a.take(8192, F32)
        B_xt_ = Buf()
        nb_ = NormBufs(a)
        hn32 = a.take(8192, F32)
        B_hn32 = Buf()
        rtb = a.take(8192, F32)
        B_rtb = Buf()
        lg = a.take(64 * 4, F32)
        B_lg = Buf()
        wgv = wg_d.ap().rearrange("e (c p) f -> e p c f", p=128)
        wuv = wu_d.ap().rearrange("e (c p) f -> e p c f", p=128)
        wdv = wd_d.ap().rearrange("e (c p) n -> e p c n", p=128)
        gcnt = [0]
        for tti in range(NTT):
            load_gain(gain_idx)
            for bi in range(TB):
                b = tti * TB + bi
                dma("sp", xt_, hsrc_d[b * 128:(b + 1) * 128, :], (bsrc,), (B_xt_,))
                if moe:
                    norm_from(nb_, xt_, B_xt_, hnTt, B_hnTt, bi, bi, fp32_out=(hn32, B_hn32))
                    for e in range(NEx):
                        dma("sp", rtb, rt_d[e:e + 1, :].partition_broadcast(128)[:, 0, :], (), (B_rtb,))
                        tt("dve", nb_.sq, hn32, rtb, ALU.mult, (B_hn32, B_rtb), (nb_.B_sq,))
                        red(lg[:, e:e + 1], nb_.sq, ALU.add, (nb_.B_sq,), (B_lg,))
                    red(lg[:, 8:9], lg[:, 0:8], ALU.max, (B_lg,), (B_lg,))
                    ts("dve", lg[:, 9:10], lg[:, 8:9], -1.0, None, ALU.mult, None, (B_lg,), (B_lg,))
                    act(lg[:, 16:24], lg[:, 0:8], AF.Exp, (B_lg,), (B_lg,), bias=lg[:, 9:10])

                    def mx(e_, o=lg[:, 24:32], i_=lg[:, 16:24]):
                        return e_.max(out=o, in_=i_)
                    sc.op("dve", mx, (B_lg,), (B_lg,))
                    ts("dve", lg[:, 32:40], lg[:, 16:24], lg[:, 25:26], None, ALU.is_ge, None, (B_lg,), (B_lg,))
                    tt("dve", lg[:, 32:40], lg[:, 32:40], lg[:, 16:24], ALU.mult, (B_lg,), (B_lg,))
                    tt("dve", lg[:, 40:41], lg[:, 24:25], lg[:, 25:26], ALU.add, (B_lg,), (B_lg,))
                    recip(lg[:, 41:42], lg[:, 40:41], (B_lg,), (B_lg,))
                    ts("dve", gates[:, b, :], lg[:, 32:40], lg[:, 41:42], None, ALU.mult, None, (B_lg,), (), (B_gates,))
                else:
                    norm_from(nb_, xt_, B_xt_, hnTt, B_hnTt, bi, bi)
            for e in range(NEx):
                for g in range(NG):
                    j = gcnt[0] % 2
                    gcnt[0] += 1
                    f0 = g * GF
                    dma("pool", wgr[j][:, 0:8, :], wgv[e, :, 0:8, f0:f0 + GF], (), (B_wgu[j],))
                    dma("pool", wgr[j][:, 8:16, :], wgv[e, :, 8:16, f0:f0 + GF], (), (), (B_wgu[j],))
                    dma("pool", wur[j][:, 0:8, :], wuv[e, :, 0:8, f0:f0 + GF], (), (), (B_wgu[j],))
                    dma("pool", wur[j][:, 8:16, :], wuv[e, :, 8:16, f0:f0 + GF], (), (), (B_wgu[j],))
                    dma("pool", wdr, wdv[e, :, g * FCN:(g + 1) * FCN, :], (), (B_wd,))
                    for hf in range(NH):
                        for fc in range(FCN):
                            pg = (hf * FCN + fc) % 2
                            pu = 2 + pg
                            for kc in range(KC):
                                mm(pss[pg], wgr[j][:, kc, fc * 128:(fc + 1) * 128], hnTt[:, kc, hf * 512:(hf + 1) * 512], kc == 0, kc == KC - 1,
                                   (B_wgu[j], B_hnTt), (B_ps[pg],))
                            for kc in range(KC):
                                mm(pss[pu], wur[j][:, kc, fc * 128:(fc + 1) * 128], hnTt[:, kc, hf * 512:(hf + 1) * 512], kc == 0, kc == KC - 1,
                                   (B_wgu[j], B_hnTt), (B_ps[pu],))
                            act(sg[pg], pss[pg], AF.Silu, (B_ps[pg],), (B_sg[pg],))
                            tt("dve", GT[j][:, fc, hf * 512:(hf + 1) * 512], pss[pu], sg[pg], ALU.mult, (B_ps[pu], B_sg[pg]),
                               (B_GT[j],) if (hf == 0 and fc == 0) else (), () if (hf == 0 and fc == 0) else (B_GT[j],))
                    first = (e == 0 and g == 0)
                    for tb in range(TB):
                        b = tti * TB + tb
                        for ct in range(4):
                            pd = 4 + ((tb * 4 + ct) % 2)
                            for fc in range(FCN):
                                mm(pss[pd], GT[j][:, fc, tb * 128:(tb + 1) * 128], wdr[:, fc, ct * 512:(ct + 1) * 512], fc == 0, fc == FCN - 1,
                                   (B_GT[j], B_wd), (B_ps[pd],))
                            oa = acc[:, tb, ct * 512:(ct + 1) * 512]
                            fw = (B_acc[tb],) if (first and ct == 0) else ()
                            fp = () if (first and ct == 0) else (B_acc[tb],)
                            if moe:
                                gcol = gates[:, b, e:e + 1]
                                if first:
                                    ts("dve", oa, pss[pd], gcol, None, ALU.mult, None, (B_ps[pd], B_gates), fw, fp)
                                else:
                                    stt(oa, pss[pd], gcol, oa, ALU.mult, ALU.add, (B_ps[pd], B_gates, B_acc[tb]), fw, fp)
                            else:
                                if first:
                                    cp("dve", oa, pss[pd], (B_ps[pd],), fw, fp)
                                else:
                                    tt("dve", oa, oa, pss[pd], ALU.add, (B_ps[pd], B_acc[tb]), fw, fp)
            if final:
                load_gain(4)
            for bi in range(TB):
                b = tti * TB + bi
                dma("sp", xt_, hsrc_d[b * 128:(b + 1) * 128, :], (bsrc,), (B_xt_,))
                tt("dve", xt_, xt_, acc[:, bi, :], ALU.add, (B_xt_, B_acc[bi]), (B_xt_,))
                if final:
                    norm_from(nb_, xt_, B_xt_, None, None, bi, bi, fp32_out=(hn32, B_hn32))
                    dma("sp", hdst_d[b * 128:(b + 1) * 128, :], hn32, (B_hn32,), (), (bdst,))
                else:
                    dma("sp", hdst_d[b * 128:(b + 1) * 128, :], xt_, (B_xt_,), (), (bdst,))

    ffn(hA_d, B_hA, hB_d, B_hB, wg0_d, wu0_d, wd0_d, 1, FF, 1, False, False)
    tapout("hB", hB_d.ap(), B_hB)
    sc.barrier()
    if stop_after == "F":
        return nc, sc, es, esem, dsem, ccsems

    a = Ar(PBASE)
    hnT = a.take(KC * TL * 2, BF16).rearrange("p (c n) -> p c n", c=KC)
    B_hnT = Buf()
    xt = [a.take(8192, F32) for _ in range(2)]
    B_xt = [Buf(), Buf()]
    nbA = NormBufs(a)
    wr = WRing(a, KC, 512)
    stg = [a.take(TL * 2, BF16) for _ in range(2)]
    B_stg = [Buf(), Buf()]
    vstg = [a.take(1024 * 2, BF16) for _ in range(2)]
    B_vstg = [Buf(), Buf()]
    fbb = a.take(64, F32)
    lfs = a.take(NB * 16 * 4, F32).rearrange("p (b h) -> p b h", h=16)
    B_fb = Buf()
    B_lfs = Buf()
    B_rcol = Buf()
    load_gain(2)
    dma("sp", fbb[:, 0:16], fb_d[0:1, :].partition_broadcast(128)[:, 0, :], (), (B_fb,))
    for b in range(NB):
        j = b % 2
        dma("sp", xt[j], hB_d[b * 128:(b + 1) * 128, :], (B_hB,), (B_xt[j],))
        norm_from(nbA, xt[j], B_xt[j], hnT, B_hnT, b, b)
    w1v = w1_d.ap().rearrange("(c p) n -> p c n", p=128)
    v1v = v1_d.ap().rearrange("(h t) d -> t h d", h=16)
    qk_groups(w1v, 0, 4, lambda head: (qt1_d[head * 128:(head + 1) * 128, :], B_qt1))
    qk_groups(w1v, 2048, 4, lambda head: (kt1_d[head * 128:(head + 1) * 128, :], B_kt1))
    v_groups(w1v, 4096, v1v, 0, B_v1, lambda kc, b: hnT[:, kc, b * 128:(b + 1) * 128], B_hnT, KC)
    v_groups(w1v, 5120, v1v, 8, B_v1, lambda kc, b: hnT[:, kc, b * 128:(b + 1) * 128], B_hnT, KC)
    wt, bw = wr.load(lambda c0, c1: w1v[:, c0:c1, 6144:6160], 16)
    for b in range(NB):
        pi = nextps()
        for kc in range(KC):
            mm(pss[pi][:, 0:16], hnT[:, kc, b * 128:(b + 1) * 128], wt[:, kc, 0:16], kc == 0, kc == KC - 1, (bw, B_hnT), (B_ps[pi],))
        tt("dve", lfs[:, b, :], pss[pi][:, 0:16], fbb[:, 0:16], ALU.add, (B_ps[pi], B_fb), (), (B_lfs,))
    act(lfs, lfs, AF.Exp, (B_lfs,), (B_lfs,), scale=-1.0)
    ts("dve", lfs, lfs, 1.0, None, ALU.add, None, (B_lfs,), (B_lfs,))
    act(lfs, lfs, AF.Ln, (B_lfs,), (B_lfs,))
    ts("dve", lfs, lfs, -1.0, None, ALU.mult, None, (B_lfs,), (B_lfs,))
    dma("sp", lf_d.ap().rearrange("(b p) h -> p b h", p=128), lfs, (B_lfs,), (B_lf,))
    tapout("qt1", qt1_d.ap(), B_qt1)
    tapout("lf", lf_d.ap(), B_lf)
    sc.barrier()
    allgather(kt1_d, kt1g_d, (B_kt1,), (B_kt1g,))
    allgather(v1_d, v1g_d, (B_v1,), (B_v1g,))
    allgather(lf_d, lfg_d, (B_lf,), (B_lfg,))
    if stop_after == "G":
        sc.barrier()
        return nc, sc, es, esem, dsem, ccsems
    attention(1)
    tapout("ot1", ot_d.ap(), B_ot)
    sc.barrier()
    if stop_after == "J":
        return nc, sc, es, esem, dsem, ccsems
    outproj(wo1_d, hB_d, B_hB, hC_d, B_hC)
    tapout("hC", hC_d.ap(), B_hC)
    sc.barrier()
    if stop_after == "K":
        return nc, sc, es, esem, dsem, ccsems
    ffn(hC_d, B_hC, y_d, B_y, wg1_d, wu1_d, wd1_d, NE, FE, 3, True, True)
    sc.barrier()
    return nc, sc, es, esem, dsem, ccsems


def gidx_of(c, TL):
    l = np.arange(TL)
    return ((l // CH) * NCORE + c) * CH + (l % CH)


def host_consts(c, S):
    TL = S // NCORE
    NB = TL // 128
    NKB = NCORE * NB
    bf = ml_dtypes.bfloat16
    p = np.arange(128)
    key_in = (p // CH) * 128 + (p % CH)
    dm = np.zeros((128, 8, 128), np.float32)
    for r in range(8):
        kpos = key_in[:, None] + r * CH
        qpos = key_in[None, :] + c * CH
        dm[:, r, :] = (kpos <= qpos).astype(np.float32)
    dmaskf = dm.reshape(128, 1024).copy()
    dmaskb = dm.astype(bf).reshape(128, 1024).copy()
    a_ = p // CH
    tri3 = np.zeros((128, 3, 128), np.float32)
    tri3[:, 0, :] = (a_[:, None] <= a_[None, :])
    tri3[:, 1, :] = (p[:, None] <= p[None, :])
    tri3[:, 2, :] = (a_[:, None] < a_[None, :])
    g = gidx_of(c, TL)
    inv = (10000.0 ** (-np.arange(0, 64, 2, dtype=np.float32) / 64)).astype(np.float32)
    ang = g.astype(np.float32)[:, None] * inv[None, :]
    cos = np.cos(ang).astype(np.float32)
    sin = np.sin(ang).astype(np.float32)
    cosT = np.concatenate([cos, cos], 1).T.copy()
    sinT = np.concatenate([-sin, sin], 1).T.copy()
    slopes = np.exp2(-8.0 * np.arange(1, 9, dtype=np.float32) / 8).astype(np.float32)
    bqd = (-slopes[:, None] * g[None, :].astype(np.float32)).astype(np.float32)
    bkd = np.zeros((128, 8, NB, 8), np.float32)
    for r in range(8):
        gr = gidx_of(r, TL).reshape(NB, 128)
        bkd[:, :, :, r] = slopes[None, :, None] * gr.T[:, None, :].astype(np.float32)
    bkd = bkd.reshape(128, 8 * NKB)
    return dict(identb=np.eye(128, dtype=np.float32).astype(bf), identf=np.eye(128, dtype=np.float32),
                dmaskb=dmaskb, dmaskf=dmaskf, dmaskn=np.ascontiguousarray((dmaskf - 1.0) * 60000.0, dtype=np.float32), tri3=tri3.reshape(128, 384).copy(), cosT=cosT, sinT=sinT,
                bqd=bqd, bkd=np.ascontiguousarray(bkd))


def host_weights(inp):
    f = lambda a: np.ascontiguousarray(np.asarray(a, dtype=np.float32))
    perm = (np.arange(64) + 32) % 64
    w_in = f(inp["ev_w_in"][0])
    w0 = np.concatenate([w_in, w_in[:, 4096 + perm]], 1)
    wuq = f(inp["ev_w_uq"][0]).reshape(512, 8, 192)
    nope = wuq[:, :, :128].reshape(512, 1024)
    pe = wuq[:, :, 128:]
    wuq_r = np.concatenate([nope, pe.reshape(512, 512), pe[:, :, perm].reshape(512, 512)], 1)
    wukv = f(inp["ev_w_ukv"][0]).reshape(512, 8, 256)
    wukv_r = np.concatenate([wukv[:, :, :128].reshape(512, 1024), wukv[:, :, 128:].reshape(512, 1024)], 1)
    gains = np.stack([f(inp["ev_attn_norm"][0]), f(inp["ev_ffn_norm"][0]), f(inp["od_attn_norm"][0]),
                      f(inp["od_ffn_norm"][0]), f(inp["final_norm"])], 0)
    lam = np.concatenate([f(inp["ev_lambda_q1"][0]), f(inp["ev_lambda_k1"][0]), f(inp["ev_lambda_q2"][0]),
                          f(inp["ev_lambda_k2"][0])])[None, :]
    return dict(
        gains=f(gains), w0=f(w0), qn=f(f(inp["ev_q_norm"][0]).reshape(4, 128).T), kvn=f(f(inp["ev_kv_norm"][0]).reshape(4, 128).T),
        wuq=f(wuq_r), wukv=f(wukv_r), lam=f(lam), subln=f(inp["ev_subln"][0])[None, :], wo0=f(inp["ev_w_out"][0]),
        wg0=f(inp["ev_ffn_w_gate"]), wu0=f(inp["ev_ffn_w_up"]), wd0=f(inp["ev_ffn_w_down"]),
        w1=f(inp["od_w_in"][0]), fb=f(inp["od_forget_bias"]), wo1=f(inp["od_w_out"][0]), rt=f(f(inp["od_router"][0]).T),
        wg1=f(inp["od_moe_w_gate"][0]), wu1=f(inp["od_moe_w_up"][0]), wd1=f(inp["od_moe_w_down"][0]))


def make_in_maps(inp, S):
    TL = S // NCORE
    x = np.asarray(inp["x"], dtype=np.float32)[0]
    hw = host_weights(inp)
    maps = []
    for c in range(NCORE):
        m = dict(hw)
        m.update(host_consts(c, S))
        m["x"] = np.ascontiguousarray(x[gidx_of(c, TL)])
        maps.append(m)
    return maps


_CACHE = {}


def kernel(**inputs):
    S = inputs["x"].shape[1]
    FF = inputs["ev_ffn_w_gate"].shape[2]
    NE = inputs["od_moe_w_gate"].shape[1]
    FE = inputs["od_moe_w_gate"].shape[3]
    TL = S // NCORE
    nc, sc, es, esem, dsem, ccsems = build(S, FF, FE, NE)
    with es:
        with nc.Block() as block:
            sc.emit(block, esem, dsem, ccsems)
    maps = make_in_maps(inputs, S)
    res = run_bass_kernel_spmd(nc, maps, core_ids=list(range(NCORE)))
    out = np.zeros((1, S, D), np.float32)
    for c in range(NCORE):
        out[0, gidx_of(c, TL)] = res.results[c]["y"]
    return out
```
